# Optimizing a Trainium2 kernel written in Bass

```python
import math
import jax, jax.numpy as jnp
from jax import lax
import numpy as np

D_MODEL = 1024
BATCH = 32
SEQ = 2048
DEPTH = 1
DEC_BATCH = 128
DEC_SEQ = 8
PAST_LEN = 8192
PAGE_SIZE = 128

HEAD_DIM = 64
N_ATTN_HEADS = 12
ATTN_WIDTH = N_ATTN_HEADS * HEAD_DIM
CONV_WIDTH = D_MODEL - ATTN_WIDTH
CONV_K = 3
BRANCHES = ((128, 1), (512, 4), (2048, 16))
WINDOW_MAX = 2048
BLK = 128
ROPE_THETA = 10000.0
NORM_EPS = 1e-6
IN_WIDTH = 3 * ATTN_WIDTH + 3 * CONV_WIDTH
PEER_HEADS = 8
PEER_NKEYS = 128
PEER_EXPERTS = PEER_NKEYS * PEER_NKEYS
PEER_QDIM = 256
PEER_TOPK = 16
PEER_CHUNK = 256
NEG = -1e30

kernel_name = 'hymba_dilated_conv_peer_step'


def rms_norm(x, g):
    xf = x.astype(jnp.float32)
    y = xf * lax.rsqrt(jnp.mean(xf * xf, axis=-1, keepdims=True) + NORM_EPS)
    return (y * g.astype(jnp.float32)).astype(x.dtype)


def rope(x, pos):
    half = HEAD_DIM // 2
    inv = jnp.exp(-math.log(ROPE_THETA) * jnp.arange(half, dtype=jnp.float32) * (2.0 / HEAD_DIM))
    ang = pos.astype(jnp.float32)[:, None] * inv[None, :]
    cos = jnp.cos(ang)[None, :, None, :]
    sin = jnp.sin(ang)[None, :, None, :]
    xf = x.astype(jnp.float32)
    x1, x2 = xf[..., :half], xf[..., half:]
    return jnp.concatenate([x1 * cos - x2 * sin, x2 * cos + x1 * sin], axis=-1).astype(x.dtype)


def softmax_parts(s):
    m = jnp.max(s, axis=-1, keepdims=True)
    p = jnp.exp(s - m)
    den = jnp.sum(p, axis=-1, keepdims=True)
    return p, den, m[..., 0] + jnp.log(den[..., 0])


def dilated_branch_prompt(q, k, v, window, dil):
    B, S, H, Dh = q.shape
    L = S // dil
    nb = -(-L // BLK)
    Lp = nb * BLK
    span = window // dil
    scale = 1.0 / math.sqrt(Dh)

    def to_sub(t):
        t = t.reshape(B, L, dil, H, Dh).transpose(0, 2, 1, 3, 4)
        t = jnp.pad(t, ((0, 0), (0, 0), (0, Lp - L), (0, 0), (0, 0)))
        return t.reshape(B, dil, nb, BLK, H, Dh)

    def with_prev(t):
        prev = jnp.pad(t, ((0, 0), (0, 0), (1, 0), (0, 0), (0, 0), (0, 0)))[:, :, :-1]
        return jnp.concatenate([prev, t], axis=3)

    qs = to_sub(q).astype(jnp.float32)
    kb = with_prev(to_sub(k)).astype(jnp.float32)
    vb = with_prev(to_sub(v)).astype(jnp.float32)
    s = jnp.einsum('brnqhd,brnkhd->brnhqk', qs, kb) * scale
    qi = jnp.arange(BLK)[:, None]
    kj = jnp.arange(2 * BLK)[None, :]
    dist = qi + BLK - kj
    kidx = jnp.arange(nb)[:, None, None] * BLK + kj[None] - BLK
    valid = (dist >= 0)[None] & (dist <= span)[None] & (kidx >= 0)
    s = jnp.where(valid[None, None, :, None], s, NEG)
    p, den, lse = softmax_parts(s)
    o = jnp.einsum('brnhqk,brnkhd->brnhqd', p, vb) / den
    o = o.transpose(0, 1, 2, 4, 3, 5).reshape(B, dil, Lp, H, Dh)[:, :, :L]
    o = o.transpose(0, 2, 1, 3, 4).reshape(B, S, H, Dh)
    lse = lse.transpose(0, 1, 2, 4, 3).reshape(B, dil, Lp, H)[:, :, :L]
    lse = lse.transpose(0, 2, 1, 3).reshape(B, S, H)
    return o, lse


def dilated_branch_sample(q, kcat, vcat, n_past, window, dil):
    T = q.shape[1]
    offs = jnp.arange(window // dil + 1) * dil
    idx = n_past + jnp.arange(T)[:, None] - offs[None, :]
    valid = idx >= 0
    idxc = jnp.maximum(idx, 0)
    kg = jnp.take(kcat, idxc, axis=1).astype(jnp.float32)
    vg = jnp.take(vcat, idxc, axis=1).astype(jnp.float32)
    s = jnp.einsum('bthd,btkhd->bthk', q.astype(jnp.float32), kg) * (1.0 / math.sqrt(HEAD_DIM))
    s = jnp.where(valid[None, :, None, :], s, NEG)
    p, den, lse = softmax_parts(s)
    o = jnp.einsum('bthk,btkhd->bthd', p, vg) / den
    return o, lse


def merge_branches(parts):
    lse = jnp.stack([l for _, l in parts], axis=0)
    wts = jax.nn.softmax(lse, axis=0)
    outs = jnp.stack([o for o, _ in parts], axis=0)
    return jnp.einsum('rbth,rbthd->bthd', wts, outs)


def short_conv(cb, cc, ch, conv_state, conv_w):
    z = cc * ch
    T = z.shape[1]
    zp = jnp.concatenate([conv_state.astype(z.dtype), z], axis=1)
    y = conv_w[0] * zp[:, 0:T]
    for i in range(1, CONV_K):
        y = y + conv_w[i] * zp[:, i:i + T]
    return cb * y, zp[:, -(CONV_K - 1):]


def peer_ffn(xn, wq, sub_keys, u, v):
    B, T, D = xn.shape
    N = B * T
    nch = -(-N // PEER_CHUNK)
    flat = jnp.pad(xn.reshape(N, D), ((0, nch * PEER_CHUNK - N), (0, 0)))

    def one(xc):
        xf = xc.astype(jnp.float32)
        q = (xc @ wq).astype(jnp.float32).reshape(-1, PEER_HEADS, 2, PEER_QDIM // 2)
        s = jnp.einsum('nhpc,hpkc->nhpk', q, sub_keys.astype(jnp.float32))
        sv, si = lax.top_k(s, PEER_TOPK)
        cand = (sv[:, :, 0, :, None] + sv[:, :, 1, None, :]).reshape(-1, PEER_HEADS, PEER_TOPK * PEER_TOPK)
        cid = (si[:, :, 0, :, None] * PEER_NKEYS + si[:, :, 1, None, :]).reshape(-1, PEER_HEADS, PEER_TOPK * PEER_TOPK)
        top, pos = lax.top_k(cand, PEER_TOPK)
        eid = jnp.take_along_axis(cid, pos, axis=-1)
        g = jax.nn.softmax(top, axis=-1)
        ue = u[eid].astype(jnp.float32)
        ve = v[eid].astype(jnp.float32)
        act = jax.nn.gelu(jnp.einsum('nd,nhkd->nhk', xf, ue), approximate=False)
        return jnp.einsum('nhk,nhkd->nd', g * act, ve).astype(xc.dtype)

    out = lax.map(one, flat.reshape(nch, PEER_CHUNK, D)).reshape(-1, D)[:N]
    return out.reshape(B, T, D)


def hybrid_layer(h, pos, conv_state, attend, norm1_g, w_in, conv_w, attn_out_g, conv_out_g,
                 w_out, norm2_g, peer_wq, peer_sub_keys, peer_u, peer_v):
    B, T, _ = h.shape
    xn = rms_norm(h, norm1_g)
    proj = xn @ w_in
    q, k, vv = [proj[..., i * ATTN_WIDTH:(i + 1) * ATTN_WIDTH].reshape(B, T, N_ATTN_HEADS, HEAD_DIM)
                for i in range(3)]
    c0 = 3 * ATTN_WIDTH
    cb, cc, ch = [proj[..., c0 + i * CONV_WIDTH:c0 + (i + 1) * CONV_WIDTH] for i in range(3)]
    q = rope(q, pos)
    k = rope(k, pos)
    attn = attend(q, k, vv).reshape(B, T, ATTN_WIDTH).astype(h.dtype)
    conv_out, conv_new = short_conv(cb, cc, ch, conv_state, conv_w)
    mixed = jnp.concatenate([rms_norm(attn, attn_out_g), rms_norm(conv_out, conv_out_g)], axis=-1) @ w_out
    h = h + mixed
    h = h + peer_ffn(rms_norm(h, norm2_g), peer_wq, peer_sub_keys, peer_u, peer_v)
    return h, k, vv, conv_new


def setup_inputs(seed: int = 0) -> dict:
    key = jax.random.key(seed)
    ks = jax.random.split(key, 20)
    f32 = jnp.float32
    win_s = min(WINDOW_MAX, PAST_LEN)
    nrm = lambda k, shape, sc: jax.random.normal(k, shape, f32) * sc
    return {
        'x_prompt': nrm(ks[0], (BATCH, SEQ, D_MODEL), 1.0),
        'x_sample': nrm(ks[1], (DEC_BATCH, DEC_SEQ, D_MODEL), 1.0),
        'cache_win_k': nrm(ks[2], (DEPTH, DEC_BATCH, win_s, N_ATTN_HEADS, HEAD_DIM), 1.0),
        'cache_win_v': nrm(ks[3], (DEPTH, DEC_BATCH, win_s, N_ATTN_HEADS, HEAD_DIM), 1.0),
        'state_conv': nrm(ks[4], (DEPTH, DEC_BATCH, CONV_K - 1, CONV_WIDTH), 1.0),
        'norm1_g': 1.0 + nrm(ks[5], (DEPTH, D_MODEL), 0.01),
        'w_in': nrm(ks[6], (DEPTH, D_MODEL, IN_WIDTH), D_MODEL ** -0.5),
        'conv_w': nrm(ks[7], (DEPTH, CONV_K, CONV_WIDTH), CONV_K ** -0.5),
        'attn_out_g': 1.0 + nrm(ks[8], (DEPTH, ATTN_WIDTH), 0.01),
        'conv_out_g': 1.0 + nrm(ks[9], (DEPTH, CONV_WIDTH), 0.01),
        'w_out': nrm(ks[10], (DEPTH, D_MODEL, D_MODEL), D_MODEL ** -0.5),
        'norm2_g': 1.0 + nrm(ks[11], (DEPTH, D_MODEL), 0.01),
        'peer_wq': nrm(ks[12], (DEPTH, D_MODEL, PEER_HEADS * PEER_QDIM), D_MODEL ** -0.5),
        'peer_sub_keys': nrm(ks[13], (DEPTH, PEER_HEADS, 2, PEER_NKEYS, PEER_QDIM // 2), (PEER_QDIM // 2) ** -0.5),
        'peer_u': nrm(ks[14], (DEPTH, PEER_EXPERTS, D_MODEL), D_MODEL ** -0.5),
        'peer_v': nrm(ks[15], (DEPTH, PEER_EXPERTS, D_MODEL), PEER_HEADS ** -0.5),
        'final_norm_g': 1.0 + nrm(ks[16], (D_MODEL,), 0.01),
    }


def reference(x_prompt, x_sample, cache_win_k, cache_win_v, state_conv, norm1_g, w_in, conv_w,
              attn_out_g, conv_out_g, w_out, norm2_g, peer_wq, peer_sub_keys, peer_u, peer_v,
              final_norm_g):
    S = x_prompt.shape[1]
    T = x_sample.shape[1]
    n_past = cache_win_k.shape[2]
    win_p = min(WINDOW_MAX, S)
    pos_p = jnp.arange(S, dtype=jnp.int32)
    pos_s = PAST_LEN + jnp.arange(T, dtype=jnp.int32)

    def attend_prompt(q, k, v):
        return merge_branches([dilated_branch_prompt(q, k, v, w, d) for w, d in BRANCHES])

    hp, hs = x_prompt, x_sample
    kp_l, vp_l, cp_l, ks_l, vs_l, cs_l = [], [], [], [], [], []
    for l in range(DEPTH):
        params = (norm1_g[l], w_in[l], conv_w[l], attn_out_g[l], conv_out_g[l], w_out[l],
                  norm2_g[l], peer_wq[l], peer_sub_keys[l], peer_u[l], peer_v[l])
        conv0 = jnp.zeros((hp.shape[0], CONV_K - 1, CONV_WIDTH), hp.dtype)
        hp, k_p, v_p, c_p = hybrid_layer(hp, pos_p, conv0, attend_prompt, *params)
        kp_l.append(k_p[:, S - win_p:])
        vp_l.append(v_p[:, S - win_p:])
        cp_l.append(c_p)
        kc, vc = cache_win_k[l], cache_win_v[l]

        def attend_sample(q, k, v, kc=kc, vc=vc):
            kcat = jnp.concatenate([kc.astype(k.dtype), k], axis=1)
            vcat = jnp.concatenate([vc.astype(v.dtype), v], axis=1)
            return merge_branches([dilated_branch_sample(q, kcat, vcat, n_past, w, d) for w, d in BRANCHES])

        hs, k_s, v_s, c_s = hybrid_layer(hs, pos_s, state_conv[l], attend_sample, *params)
        ks_l.append(k_s)
        vs_l.append(v_s)
        cs_l.append(c_s)
    y_prompt = rms_norm(hp, final_norm_g)
    y_sample = rms_norm(hs, final_norm_g)
    return (y_prompt, y_sample, jnp.stack(kp_l), jnp.stack(vp_l), jnp.stack(cp_l),
            jnp.stack(ks_l), jnp.stack(vs_l), jnp.stack(cs_l))
```

```python
import contextlib
import math
import numpy as np
import ml_dtypes
import concourse.bass as bass
import concourse.mybir as mybir
from concourse.bass_utils import run_bass_kernel_spmd

F32 = mybir.dt.float32
BF16 = mybir.dt.bfloat16
I32 = mybir.dt.int32
U32 = mybir.dt.uint32
AF = mybir.ActivationFunctionType
ALU = mybir.AluOpType
AX = mybir.AxisListType

NCORES = 8
D = 1024
SEQ = 2048
NSEQ = 4
NSB = 16
NT = SEQ // 128
EPS = 1e-6
NEGBIG = -1e30


class Op:
    __slots__ = ("eng", "fn", "reads", "writes", "key", "deps", "sig", "ticket", "waits", "xdeps")

    def __init__(self, eng, fn, reads, writes, key, xdeps):
        self.eng, self.fn, self.reads, self.writes, self.key = eng, fn, reads, writes, key
        self.xdeps = xdeps
        self.deps, self.sig, self.ticket, self.waits = set(), False, None, []


class Sched:
    ENGS = ("sp", "act", "pe", "dve", "pool")

    def __init__(self):
        self.ops = []

    def add(self, eng, fn, r=(), w=(), key=None, xdeps=()):
        op = Op(eng, fn, tuple(r), tuple(w), key, tuple(xdeps))
        self.ops.append(op)
        return len(self.ops) - 1

    def dma(self, eng, fn, r=(), w=(), key=None):
        assert key is not None
        return self.add(eng, fn, r, w, key)

    def barrier(self):
        last = {}
        for i, op in enumerate(self.ops):
            if op.key is not None:
                last[("k", op.key)] = i
            elif op.fn is not None:
                last[("e", op.eng)] = i
        deps = tuple(last.values())
        for e in self.ENGS:
            self.add(e, None, xdeps=deps)

    def finalize(self):
        ops = self.ops
        last_w, readers = {}, {}
        for i, op in enumerate(ops):
            d = set(op.xdeps)
            for r in op.reads:
                if r in last_w:
                    d.add(last_w[r])
            for w in op.writes:
                if w in last_w:
                    d.add(last_w[w])
                d.update(readers.get(w, ()))
            d.discard(i)
            op.deps = d
            for r in op.reads:
                readers.setdefault(r, []).append(i)
            for w in op.writes:
                last_w[w] = i
                readers[w] = []
        for op in ops:
            for j in op.deps:
                dj = ops[j]
                if dj.key is not None:
                    continue
                if dj.fn is None:
                    continue
                if dj.eng != op.eng or op.eng != "pe":
                    dj.sig = True
        ecount = {e: 0 for e in self.ENGS}
        kcount = {}
        for op in ops:
            if op.key is not None:
                kcount[op.key] = kcount.get(op.key, 0) + 16
                op.ticket = (("k", op.key), kcount[op.key])
            elif op.sig:
                ecount[op.eng] += 1
                op.ticket = (("e", op.eng), ecount[op.eng])
        waited = {e: {} for e in self.ENGS}
        for op in ops:
            need = {}
            for j in op.deps:
                dj = ops[j]
                if dj.ticket is None:
                    continue
                if dj.key is None and dj.eng == op.eng and op.eng == "pe":
                    continue
                s, v = dj.ticket
                if v > need.get(s, 0):
                    need[s] = v
            wl = []
            for s, v in need.items():
                if waited[op.eng].get(s, 0) >= v:
                    continue
                waited[op.eng][s] = v
                wl.append((s, v))
            op.waits = wl
        self.keys = sorted(kcount.keys())

    def emit(self, nc, stack):
        self.finalize()
        sems = {}
        for e in self.ENGS:
            sems[("e", e)] = stack.enter_context(nc.semaphore("se_" + e))
        for k in self.keys:
            sems[("k", k)] = stack.enter_context(nc.semaphore("sk_" + k))
        block = stack.enter_context(nc.Block())
        ops = self.ops

        def run(engname):
            def body(e):
                for op in ops:
                    if op.eng != engname:
                        continue
                    for s, v in op.waits:
                        e.wait_ge(sems[s], v)
                    if op.fn is None:
                        continue
                    ins = op.fn(e)
                    if op.ticket is not None:
                        ins.then_inc(sems[op.ticket[0]], 16 if op.key is not None else 1)
            return body

        block.sync(run("sp"))
        block.scalar(run("act"))
        block.tensor(run("pe"))
        block.vector(run("dve"))
        block.gpsimd(run("pool"))


def _mult(delta):
    delta = np.asarray(delta)
    m = ((delta >= 0) & (delta <= 128)).astype(np.float32)
    m += ((delta >= 0) & (delta % 4 == 0) & (delta <= 512)).astype(np.float32)
    m += ((delta >= 0) & (delta % 16 == 0) & (delta <= 2048)).astype(np.float32)
    return m


def _rope_tables(pos):
    half = 32
    inv = np.exp(-math.log(10000.0) * np.arange(half, dtype=np.float32) * np.float32(2.0 / 64)).astype(np.float32)
    ang = pos.astype(np.float32)[:, None] * inv[None, :]
    c, s = np.cos(ang).astype(np.float32), np.sin(ang).astype(np.float32)
    C = np.concatenate([c, c], axis=1)
    S = np.concatenate([-s, s], axis=1)
    return np.stack([C, S], axis=1).astype(np.float32)


def make_consts():
    c = {}
    c["ident_bf"] = np.eye(128, dtype=np.float32).astype(ml_dtypes.bfloat16)
    c["rope_p"] = _rope_tables(np.arange(SEQ))
    c["rope_s"] = _rope_tables(8192 + (np.arange(128) % 8))
    pk = np.arange(128)[:, None, None]
    pos = np.arange(17)[None, :, None]
    pq = np.arange(128)[None, None, :]
    c["mask_p"] = _mult(128 * (16 - pos) + pq - pk).astype(ml_dtypes.bfloat16)
    j = np.arange(16)[None, :, None]
    t = np.arange(8)[None, None, :]
    c["mask_sc"] = _mult(2048 + t - (128 * j + pk)).astype(ml_dtypes.bfloat16)
    kb, kt = np.arange(128)[:, None] // 8, np.arange(128)[:, None] % 8
    qb, qt = np.arange(128)[None, :] // 8, np.arange(128)[None, :] % 8
    c["mask_sn"] = (_mult(qt - kt) * (kb == qb)).astype(ml_dtypes.bfloat16)
    cm = np.zeros((128, 9, 128), np.float32)
    tp = np.arange(128)[:, None]
    tt = np.arange(128)[None, :]
    cm[:, 0] = (tp == tt - 1)
    cm[:, 1] = (tp == 127) & (tt == 0)
    cm[:, 2] = (tp == tt - 2)
    cm[:, 3] = ((tp == 126) & (tt == 0)) | ((tp == 127) & (tt == 1))
    cm[:, 4] = (tp == tt - 1) & (tt % 8 >= 1)
    cm[:, 5] = (tp == tt - 2) & (tt % 8 >= 2)
    rb, rr = tp // 2, tp % 2
    cm[:, 6] = (tp < 32) & (rb == tt // 8) & (rr == 1) & (tt % 8 == 0)
    cm[:, 7] = (tp < 32) & (rb == tt // 8) & (((rr == 0) & (tt % 8 == 0)) | ((rr == 1) & (tt % 8 == 1)))
    cm[:, 8] = (tt < 32) & (tp == 8 * (tt // 2) + 6 + (tt % 2))
    c["cmat"] = cm
    w = np.zeros((8, 256), np.float32)
    w[np.arange(8), 128 + np.arange(8)] = 1.0
    c["wshift"] = w
    c["iota16"] = np.tile(np.arange(16, dtype=np.float32)[None, :], (128, 1))
    return c


CONST_SPECS = [
    ("ident_bf", [128, 128], BF16), ("rope_p", [SEQ, 2, 64], F32), ("rope_s", [128, 2, 64], F32),
    ("mask_p", [128, 17, 128], BF16), ("mask_sc", [128, 16, 8], BF16), ("mask_sn", [128, 128], BF16),
    ("cmat", [128, 9, 128], F32), ("wshift", [8, 256], F32), ("iota16", [128, 16], F32),
]

IN_SPECS = [
    ("xp", [NSEQ, SEQ, D], F32), ("xs", [128, D], F32),
    ("ck", [NSB, 2048, 768], F32), ("cv", [NSB, 2048, 768], F32), ("sc", [32, 256], F32),
    ("g1", [D], F32), ("w_in", [D, 3072], F32), ("conv_w", [3, 256], F32), ("go", [D], F32),
    ("w_out", [D, D], F32), ("g2", [D], F32), ("wq", [D, 2048], F32), ("sk", [16, 128, 128], F32),
    ("pu", [16384, D], F32), ("pv", [16384, D], F32), ("gf", [D], F32),
]

OUT_SPECS = [
    ("yp", [NSEQ, SEQ, D]), ("ys", [128, D]), ("kp", [NSEQ, SEQ, 768]), ("vp", [NSEQ, SEQ, 768]),
    ("cp", [NSEQ, 2, 256]), ("ks", [128, 768]), ("vs", [128, 768]), ("cs", [32, 256]),
]


def bcast_rows(ap1d, n):
    return ap1d.partition_broadcast(128)


def build(n_ptiles=NSEQ * NT, do_sample=True, do_phase2=True, debug_h1=False, small_io=False, max_ops=None, skip_ops=(), nsb_run=NSB, sample_front_only=False):
    nc = bass.Bass("TRN2", target_bir_lowering=False)
    A = {}
    for name, shape, dt in IN_SPECS + CONST_SPECS:
        if small_io and name in ("pu", "pv"):
            shape = [512, D]
        if small_io and name in ("ck", "cv"):
            shape = [max(1, nsb_run), 2048, 768]
        if small_io and name == "xp":
            shape = [1, 256, D]
        A[name] = nc.dram_tensor(name, shape, dt, kind="ExternalInput").ap()
    for name, shape in OUT_SPECS:
        if small_io and name in ("yp", "kp", "vp"):
            shape = [1, 256, shape[2]]
        A[name] = nc.dram_tensor(name, shape, F32, kind="ExternalOutput").ap()
    ntile_all = NSEQ * NT + 1
    h1d = nc.dram_tensor("h1d", [ntile_all, 128, D], F32,
                         kind="ExternalOutput" if debug_h1 else "Internal").ap()
    uvd = nc.dram_tensor("uvd", [16384, 2048], BF16, kind="Internal").ap()

    S = Sched()
    with contextlib.ExitStack() as st:
        st.enter_context(nc.allow_low_precision("bf16 matmul operands, fp32 accumulation"))
        st.enter_context(nc.allow_non_contiguous_dma("small strided constant loads"))

        def sb(name, shape, dt):
            return st.enter_context(nc.sbuf_tensor(name, shape, dt))

        psf = [st.enter_context(nc.psum_tensor(f"psf{i}", [128, 512], F32)) for i in range(6)]
        psb = [st.enter_context(nc.psum_tensor(f"psb{i}", [128, 1024], BF16)) for i in range(2)]
        PSF = [f"ps{i}" for i in range(6)]
        PSB = ["ps6", "ps7"]

        ident = sb("ident", [128, 128], BF16)
        cmat = sb("cmat_sb", [128, 9, 128], F32)
        wshift = sb("wshift_sb", [8, 256], F32)
        S.dma("sp", lambda e: e.dma_start(out=ident[:, :], in_=A["ident_bf"]), w=["ident"], key="cu1")
        S.dma("sp", lambda e: e.dma_start(out=cmat[:, :, :], in_=A["cmat"]), w=["cmat"], key="cu2")
        S.dma("sp", lambda e: e.dma_start(out=wshift[:, :], in_=A["wshift"]), w=["wshift"], key="cu3")

        p1 = contextlib.ExitStack()
        st.enter_context(p1)

        def sb1(name, shape, dt):
            return p1.enter_context(nc.sbuf_tensor(name, shape, dt))

        w_in_bf = sb1("w_in_bf", [128, 8, 3072], BF16)
        w_out_bf = sb1("w_out_bf", [128, 8, 1024], BF16)
        g1b = sb1("g1b", [128, D], BF16)
        gob = sb1("gob", [128, D], BF16)
        cwb = sb1("cwb", [128, 3, 256], F32)
        mask_p = sb1("mask_p_sb", [128, 17, 128], BF16)
        mask_sc = sb1("mask_sc_sb", [128, 16, 8], BF16)
        mask_sn = sb1("mask_sn_sb", [128, 128], BF16)
        SEQB = sb1("seqbuf", [128, 37056], BF16)
        KT = SEQB[:, 0:12288].rearrange("p (a t) -> p a t", a=6)
        VA = SEQB[:, 12288:24768].rearrange("p (j h c) -> p j h c", j=16, h=12)
        Kb = SEQB[:, 0:12288].rearrange("p (j c) -> p j c", j=16)
        KTb = SEQB[:, 12288:24576].rearrange("p (a j t) -> p a j t", a=6, j=16)
        Vb = SEQB[:, 24768:37056].rearrange("p (j c) -> p j c", j=16)
        ones_c = sb1("ones_c", [128, 2], BF16)

        xin = [sb1(f"xin{i}", [128, D], F32) for i in range(2)]
        ropet = [sb1(f"ropet{i}", [128, 2, 64], F32) for i in range(2)]
        xb = sb1("xb", [128, D], BF16)
        TT = sb1("TT", [128, 8, 128], BF16)
        junk = sb1("junk", [128, D], BF16)
        stat = sb1("stat", [128, 16], F32)
        qk = sb1("qk", [128, 1536], F32)
        rtmp = sb1("rtmp", [128, 780], F32)
        qkb = sb1("qkb", [128, 1536], BF16)
        vf = sb1("vf", [128, 768], F32)
        cf = sb1("cf", [128, 768], F32)
        zb = [sb1(f"zb{i}", [128, 256], F32) for i in range(2)]
        yc = sb1("yc", [128, 256], F32)
        ctmp = sb1("ctmp", [128, 256], F32)
        QT = sb1("QT", [128, 6, 128], BF16)
        KTs = xb[:, 0:768].rearrange("p (a t) -> p a t", a=6)
        VAs = junk[:, 0:780].rearrange("p (h c) -> p h c", c=65)
        Eb = [sb1(f"Eb{i}", [128, 4, 128], BF16) for i in range(2)]
        PT = [sb1(f"PT{i}", [128, 4, 128], BF16) for i in range(2)]
        rden = sb1("rden", [128, 12], F32)
        attn = sb1("attn", [128, 768], F32)
        mixin = sb1("mixin", [128, D], BF16)
        scst = sb1("scst", [32, 256], F32)
        obs = rtmp[0:8, 0:780]
        csout = sb1("csout", [32, 256], F32)

        for kc in range(8):
            S.dma("pool", lambda e, kc=kc: e.dma_start(out=w_in_bf[:, kc, :], in_=A["w_in"][kc * 128:(kc + 1) * 128, :]),
                  w=["w_in_bf"], key=f"wl{kc % 4}")
        for kc in range(8):
            S.dma("pool", lambda e, kc=kc: e.dma_start(out=w_out_bf[:, kc, :], in_=A["w_out"][kc * 128:(kc + 1) * 128, :]),
                  w=["w_out_bf"], key=f"wo{kc % 4}")
        S.dma("pool", lambda e: e.dma_start(out=g1b[:, :], in_=A["g1"].partition_broadcast(128)), w=["g1b"], key="cu4")
        S.dma("pool", lambda e: e.dma_start(out=gob[:, :], in_=A["go"].partition_broadcast(128)), w=["gob"], key="cu5")
        S.dma("sp", lambda e: e.dma_start(out=cwb[:, :, :].rearrange("p a c -> p (a c)"),
                                          in_=A["conv_w"].rearrange("a c -> (a c)").partition_broadcast(128)),
              w=["cwb"], key="cu6")
        S.dma("sp", lambda e: e.dma_start(out=mask_p[:, :, :], in_=A["mask_p"]), w=["mask_p"], key="cu7")
        S.dma("sp", lambda e: e.dma_start(out=mask_sc[:, :, :], in_=A["mask_sc"]), w=["mask_sc"], key="cu8")
        S.dma("sp", lambda e: e.dma_start(out=mask_sn[:, :], in_=A["mask_sn"]), w=["mask_sn"], key="cu9")
        S.add("dve", lambda e: e.memset(ones_c[:, :], 1.0), w=["ones_c"])
        S.add("dve", lambda e: e.memset(SEQB[:, 12288:24768], 1.0), w=[f"VA{j}" for j in range(16)])

        def rstd_ops(ssq_col, out_col, n):
            S.add("act", lambda e: e.activation(out=stat[:, 15:16], in_=stat[:, ssq_col:ssq_col + 1], func=AF.Ln,
                                                bias=EPS, scale=1.0 / n), r=[f"st{ssq_col}"], w=["st15"])
            S.add("act", lambda e: e.activation(out=stat[:, out_col:out_col + 1], in_=stat[:, 15:16], func=AF.Exp,
                                                scale=-0.5), r=["st15"], w=[f"st{out_col}"])

        def front(src_ap, slot, rope_src, is_sample, ti, kdst, vdst):
            X = xin[slot]
            xn = f"xin{slot}"
            rt = ropet[slot]
            rn = f"ropet{slot}"
            S.dma("sp", lambda e: e.dma_start(out=X[:, :], in_=src_ap), w=[xn], key=xn)
            S.dma("sp", lambda e: e.dma_start(out=rt[:, :, :], in_=rope_src), w=[rn], key=rn)
            S.add("dve", lambda e: e.scalar_tensor_tensor(out=junk[:, :], in0=X[:, :], scalar=1.0, in1=X[:, :],
                                                          op0=ALU.mult, op1=ALU.mult, accum_out=stat[:, 0:1]),
                  r=[xn], w=["junk", "st0"])
            rstd_ops(0, 1, D)
            S.add("dve", lambda e: e.tensor_tensor(out=xb[:, :], in0=X[:, :], in1=g1b[:, :], op=ALU.mult),
                  r=[xn, "g1b"], w=["xb"])
            for kc in range(8):
                S.add("pe", lambda e, kc=kc: e.transpose(out=psb[0][:, kc * 128:(kc + 1) * 128],
                                                         in_=xb[:, kc * 128:(kc + 1) * 128], identity=ident[:, :]),
                      r=["xb", "ident"], w=["ps6"])
            S.add("act", lambda e: e.activation(out=TT[:, :, :].rearrange("p a t -> p (a t)"), in_=psb[0][:, :],
                                                func=AF.Copy), r=["ps6"], w=["TT"])
            for nb in range(6):
                for kc in range(8):
                    S.add("pe", lambda e, nb=nb, kc=kc: e.matmul(psf[nb][:, :], lhsT=TT[:, kc, :],
                                                                 rhs=w_in_bf[:, kc, nb * 512:(nb + 1) * 512],
                                                                 start=(kc == 0), stop=(kc == 7)),
                          r=["TT", "w_in_bf"], w=[PSF[nb]])
            for nb in range(3):
                S.add("act", lambda e, nb=nb: e.activation(out=qk[:, nb * 512:(nb + 1) * 512], in_=psf[nb][:, :],
                                                           func=AF.Copy, scale=stat[:, 1:2]),
                      r=[PSF[nb], "st1"], w=[["qk0"], ["qk0", "qk1"], ["qk1"]][nb])
            S.add("act", lambda e: e.activation(out=vf[:, 0:512], in_=psf[3][:, :], func=AF.Copy, scale=stat[:, 1:2]),
                  r=[PSF[3], "st1"], w=["vfa"])
            S.add("act", lambda e: e.activation(out=vf[:, 512:768], in_=psf[4][:, 0:256], func=AF.Copy, scale=stat[:, 1:2]),
                  r=[PSF[4], "st1"], w=["vfb"])
            S.add("act", lambda e: e.activation(out=cf[:, 0:256], in_=psf[4][:, 256:512], func=AF.Copy,
                                                scale=stat[:, 1:2]), r=[PSF[4], "st1"], w=["cf"])
            S.add("act", lambda e: e.activation(out=cf[:, 256:768], in_=psf[5][:, :], func=AF.Copy,
                                                scale=stat[:, 1:2]), r=[PSF[5], "st1"], w=["cf"])
            Cb = rt[:, 0:1, :].to_broadcast([128, 12, 64])
            S1 = rt[:, 1:2, 0:32].to_broadcast([128, 12, 32])
            S2 = rt[:, 1:2, 32:64].to_broadcast([128, 12, 32])
            for hf in range(2):
                qh = qk[:, hf * 768:(hf + 1) * 768]
                q4 = qh.rearrange("p (h c) -> p h c", c=64)
                r4 = rtmp[:, 0:768].rearrange("p (h c) -> p h c", c=64)
                S.add("pool", lambda e, q4=q4, r4=r4: e.tensor_tensor(out=r4[:, :, 0:32], in0=q4[:, :, 32:64], in1=S1, op=ALU.mult),
                      r=[f"qk{hf}", rn], w=["rtmpa"])
                S.add("pool", lambda e, q4=q4, r4=r4: e.tensor_tensor(out=r4[:, :, 32:64], in0=q4[:, :, 0:32], in1=S2, op=ALU.mult),
                      r=[f"qk{hf}", rn], w=["rtmpb"])
                S.add("dve", lambda e, q4=q4: e.tensor_tensor(out=q4, in0=q4, in1=Cb, op=ALU.mult), r=[f"qk{hf}", rn], w=[f"qk{hf}"])
                S.add("dve", lambda e, qh=qh: e.tensor_tensor(out=qh, in0=qh, in1=rtmp[:, 0:768], op=ALU.add),
                      r=[f"qk{hf}", "rtmpa", "rtmpb"], w=[f"qk{hf}"])
            S.add("act", lambda e: e.activation(out=qkb[:, :], in_=qk[:, :], func=AF.Copy), r=["qk0", "qk1"], w=["qkb"])
            S.dma("sp", lambda e: e.dma_start(out=kdst, in_=qk[:, 768:1536]), r=["qk1"], key="kout")
            S.dma("sp", lambda e: e.dma_start(out=vdst, in_=vf[:, :]), r=["vfa", "vfb"], key="vout")
            for hp in range(6):
                S.add("pe", lambda e, hp=hp: e.transpose(out=psb[0][:, hp * 128:(hp + 1) * 128],
                                                         in_=qkb[:, hp * 128:(hp + 1) * 128], identity=ident[:, :]),
                      r=["qkb", "ident"], w=["ps6"])
                S.add("pe", lambda e, hp=hp: e.transpose(out=psb[1][:, hp * 128:(hp + 1) * 128],
                                                         in_=qkb[:, 768 + hp * 128:768 + (hp + 1) * 128],
                                                         identity=ident[:, :]),
                      r=["qkb", "ident"], w=["ps7"])
            S.add("act", lambda e: e.activation(out=QT[:, :, :].rearrange("p a t -> p (a t)"), in_=psb[0][:, 0:768],
                                                func=AF.Copy), r=["ps6"], w=["QT"])
            if is_sample:
                S.add("dve", lambda e: e.tensor_copy(out=KTs, in_=psb[1][:, 0:768].rearrange("p (a t) -> p a t", a=6)),
                      r=["ps7"], w=["KTs", "xb"])
                S.add("dve", lambda e: e.memset(junk[:, 0:780], 1.0), w=["VAs", "junk"])
                S.add("act", lambda e: e.activation(out=VAs[:, :, 0:64], in_=vf[:, :].rearrange("p (h c) -> p h c", c=64),
                                                    func=AF.Copy), r=["vfa", "vfb"], w=["VAs", "junk"])
            else:
                S.add("dve", lambda e: e.tensor_copy(out=KT[:, :, ti * 128:(ti + 1) * 128],
                                                     in_=psb[1][:, 0:768].rearrange("p (a t) -> p a t", a=6)),
                      r=["ps7"], w=[f"KT{ti}"])
                S.add("act", lambda e: e.activation(out=VA[:, ti, :, 0:64], in_=vf[:, :].rearrange("p (h c) -> p h c", c=64),
                                                    func=AF.Copy), r=["vfa", "vfb"], w=[f"VA{ti}"])

        def conv(zcur, zprev, zn_cur, zn_prev, mats, prev_parts):
            S.add("dve", lambda e: e.tensor_tensor(out=zcur[:, :], in0=cf[:, 256:512], in1=cf[:, 512:768], op=ALU.mult),
                  r=["cf"], w=[zn_cur])
            a1, b1, a2, b2 = mats
            hasprev = zprev is not None
            S.add("pe", lambda e: e.matmul(psf[4][:, 0:256], lhsT=cmat[:, a1, :], rhs=zcur[:, :], start=True, stop=not hasprev),
                  r=["cmat", zn_cur], w=[PSF[4]])
            if hasprev:
                S.add("pe", lambda e: e.matmul(psf[4][:, 0:256], lhsT=cmat[0:prev_parts, b1, :], rhs=zprev[0:prev_parts, :],
                                               start=False, stop=True), r=["cmat", zn_prev], w=[PSF[4]])
            S.add("pe", lambda e: e.matmul(psf[5][:, 0:256], lhsT=cmat[:, a2, :], rhs=zcur[:, :], start=True, stop=not hasprev),
                  r=["cmat", zn_cur], w=[PSF[5]])
            if hasprev:
                S.add("pe", lambda e: e.matmul(psf[5][:, 0:256], lhsT=cmat[0:prev_parts, b2, :], rhs=zprev[0:prev_parts, :],
                                               start=False, stop=True), r=["cmat", zn_prev], w=[PSF[5]])
            S.add("dve", lambda e: e.tensor_tensor(out=yc[:, :], in0=zcur[:, :], in1=cwb[:, 2, :], op=ALU.mult),
                  r=[zn_cur, "cwb"], w=["yc"])
            S.add("dve", lambda e: e.tensor_tensor(out=ctmp[:, :], in0=psf[4][:, 0:256], in1=cwb[:, 1, :], op=ALU.mult),
                  r=[PSF[4], "cwb"], w=["ctmp"])
            S.add("dve", lambda e: e.tensor_tensor(out=yc[:, :], in0=yc[:, :], in1=ctmp[:, :], op=ALU.add),
                  r=["yc", "ctmp"], w=["yc"])
            S.add("dve", lambda e: e.tensor_tensor(out=ctmp[:, :], in0=psf[5][:, 0:256], in1=cwb[:, 0, :], op=ALU.mult),
                  r=[PSF[5], "cwb"], w=["ctmp"])
            S.add("dve", lambda e: e.tensor_tensor(out=yc[:, :], in0=yc[:, :], in1=ctmp[:, :], op=ALU.add),
                  r=["yc", "ctmp"], w=["yc"])
            S.add("dve", lambda e: e.tensor_tensor(out=yc[:, :], in0=yc[:, :], in1=cf[:, 0:256], op=ALU.mult),
                  r=["yc", "cf"], w=["yc"])

        def attn_groups(items):
            def scores(g, it):
                pb = g % 2
                for jj, (lk, rq, rn_) in enumerate(it["sc"]):
                    S.add("pe", lambda e, lk=lk, rq=rq, jj=jj, pb=pb: e.matmul(psf[pb][:, jj * 128:(jj + 1) * 128], lhsT=lk, rhs=rq,
                                                                              start=True, stop=True),
                          r=rn_, w=[PSF[pb]])
                n = len(it["sc"])
                S.add("act", lambda e, pb=pb, n=n: e.activation(out=Eb[pb][:, 0:n, :].rearrange("p a t -> p (a t)"),
                                                                 in_=psf[pb][:, 0:n * 128], func=AF.Exp, scale=0.125),
                      r=[PSF[pb]], w=[f"Eb{pb}"])
                S.add("dve", lambda e, pb=pb, n=n, it=it: e.tensor_tensor(out=PT[pb][:, 0:n, :], in0=Eb[pb][:, 0:n, :],
                                                                          in1=it["mask"], op=ALU.mult),
                      r=[f"Eb{pb}", it["maskn"]], w=[f"PT{pb}"])

            def pvs(g, it):
                pb = g % 2
                for (outap, jj, rhs, start, stop, rnames, wname) in it["pv"]:
                    S.add("pe", lambda e, outap=outap, jj=jj, rhs=rhs, start=start, stop=stop, pb=pb:
                          e.matmul(outap, lhsT=PT[pb][:, jj, :], rhs=rhs, start=start, stop=stop),
                          r=[f"PT{pb}"] + rnames, w=[wname])
            for g, it in enumerate(items):
                scores(g, it)
                if g >= 1:
                    pvs(g - 1, items[g - 1])
            if items:
                pvs(len(items) - 1, items[-1])

        def attn_finish():
            for bk in range(2):
                O = psf[2 + bk][:, 0:390].rearrange("p (h c) -> p h c", c=65)
                S.add("dve", lambda e, O=O, bk=bk: e.reciprocal(out=rden[:, bk * 6:(bk + 1) * 6], in_=O[:, :, 64]),
                      r=[PSF[2 + bk]], w=[f"rden{bk}"])
                S.add("dve", lambda e, O=O, bk=bk: e.tensor_tensor(
                    out=attn[:, bk * 384:(bk + 1) * 384].rearrange("p (h c) -> p h c", c=64), in0=O[:, :, 0:64],
                    in1=rden[:, bk * 6:(bk + 1) * 6].unsqueeze(2).to_broadcast([128, 6, 64]), op=ALU.mult),
                    r=[PSF[2 + bk], f"rden{bk}"], w=[f"attn{bk}"])

        def back(slot, gidx):
            X = xin[slot]
            xn = f"xin{slot}"
            S.add("dve", lambda e: e.scalar_tensor_tensor(out=junk[:, 0:768], in0=attn[:, :], scalar=1.0, in1=attn[:, :],
                                                          op0=ALU.mult, op1=ALU.mult, accum_out=stat[:, 2:3]),
                  r=["attn0", "attn1"], w=["junk", "st2"])
            rstd_ops(2, 3, 768)
            S.add("dve", lambda e: e.scalar_tensor_tensor(out=junk[:, 0:256], in0=yc[:, :], scalar=1.0, in1=yc[:, :],
                                                          op0=ALU.mult, op1=ALU.mult, accum_out=stat[:, 4:5]),
                  r=["yc"], w=["junk", "st4"])
            rstd_ops(4, 5, 256)
            S.add("dve", lambda e: e.scalar_tensor_tensor(out=mixin[:, 0:768], in0=attn[:, :], scalar=stat[:, 3:4],
                                                          in1=gob[:, 0:768], op0=ALU.mult, op1=ALU.mult),
                  r=["attn0", "attn1", "st3", "gob"], w=["mixa"])
            S.add("dve", lambda e: e.scalar_tensor_tensor(out=mixin[:, 768:1024], in0=yc[:, :], scalar=stat[:, 5:6],
                                                          in1=gob[:, 768:1024], op0=ALU.mult, op1=ALU.mult),
                  r=["yc", "st5", "gob"], w=["mixb"])
            for kc in range(8):
                S.add("pe", lambda e, kc=kc: e.transpose(out=psb[0][:, kc * 128:(kc + 1) * 128],
                                                         in_=mixin[:, kc * 128:(kc + 1) * 128], identity=ident[:, :]),
                      r=["mixa", "mixb", "ident"], w=["ps6"])
            S.add("act", lambda e: e.activation(out=TT[:, :, :].rearrange("p a t -> p (a t)"), in_=psb[0][:, :],
                                                func=AF.Copy), r=["ps6"], w=["TT"])
            for nb in range(2):
                for kc in range(8):
                    S.add("pe", lambda e, nb=nb, kc=kc: e.matmul(psf[4 + nb][:, :], lhsT=TT[:, kc, :],
                                                                 rhs=w_out_bf[:, kc, nb * 512:(nb + 1) * 512],
                                                                 start=(kc == 0), stop=(kc == 7)),
                          r=["TT", "w_out_bf"], w=[PSF[4 + nb]])
            for nb in range(2):
                S.add("dve", lambda e, nb=nb: e.tensor_tensor(out=X[:, nb * 512:(nb + 1) * 512], in0=psf[4 + nb][:, :],
                                                              in1=X[:, nb * 512:(nb + 1) * 512], op=ALU.add),
                      r=[PSF[4 + nb], xn], w=[xn])
            S.dma("sp", lambda e: e.dma_start(out=h1d[gidx, :, :], in_=X[:, :]), r=[xn], w=[f"h1d{gidx}"], key=f"h1o{slot}")

        for g in range(n_ptiles):
            s, ti = divmod(g, NT)
            slot = g % 2
            front(A["xp"][s, ti * 128:(ti + 1) * 128, :], slot, A["rope_p"][ti * 128:(ti + 1) * 128, :, :], False, ti,
                  A["kp"][s, ti * 128:(ti + 1) * 128, :], A["vp"][s, ti * 128:(ti + 1) * 128, :])
            zc, zp = zb[g % 2], zb[(g + 1) % 2]
            conv(zc, zp if ti > 0 else None, f"zb{g % 2}", f"zb{(g + 1) % 2}", (0, 1, 2, 3), 128)
            if ti == NT - 1:
                S.dma("sp", lambda e, s=s, zc=zc: e.dma_start(out=A["cp"][s, :, :], in_=zc[126:128, :]),
                      r=[f"zb{g % 2}"], key="cpo")
            items = []
            for h in range(12):
                hp, base = h // 2, 64 * (h % 2)
                bank, hh = 2 + h // 6, h % 6
                for g0 in range(0, ti + 1, 4):
                    js = list(range(g0, min(g0 + 4, ti + 1)))
                    it = {"sc": [], "pv": []}
                    for j in js:
                        it["sc"].append((KT[base:base + 64, hp, j * 128:(j + 1) * 128], QT[base:base + 64, hp, :],
                                         [f"KT{j}", "QT"]))
                    p0 = 16 - ti + js[0]
                    it["mask"] = mask_p[:, p0:p0 + len(js), :]
                    it["maskn"] = "mask_p"
                    for jj, j in enumerate(js):
                        it["pv"].append((psf[bank][:, hh * 65:(hh + 1) * 65], jj, VA[:, j, h, :], j == 0, j == ti,
                                         [f"VA{j}"], PSF[bank]))
                    items.append(it)
            attn_groups(items)
            attn_finish()
            back(slot, g)

        if do_sample:
            g = NSEQ * NT
            slot = g % 2
            S.dma("sp", lambda e: e.dma_start(out=scst[:, :], in_=A["sc"]), w=["scst"], key="cu10")
            front(A["xs"], slot, A["rope_s"], True, 0, A["ks"], A["vs"])
            zc = zb[0]
            conv(zc, scst, "zb0", "scst", (4, 6, 5, 7), 32)
            S.add("pe", lambda e: e.matmul(psf[4][0:32, 256:512], lhsT=cmat[:, 8, 0:32], rhs=zc[:, :], start=True, stop=True),
                  r=["cmat", "zb0"], w=[PSF[4]])
            S.add("act", lambda e: e.activation(out=csout[:, :], in_=psf[4][0:32, 256:512], func=AF.Copy), r=[PSF[4]], w=["csout"])
            S.dma("sp", lambda e: e.dma_start(out=A["cs"], in_=csout[:, :]), r=["csout"], key="cso")
            if not sample_front_only:
              for b in range(nsb_run):
                  for q4 in range(4):
                      S.dma("pool", lambda e, b=b, q4=q4: e.dma_start(
                          out=Kb[:, q4 * 4:(q4 + 1) * 4, :],
                          in_=A["ck"][b, q4 * 512:(q4 + 1) * 512, :].rearrange("(j p) c -> p j c", p=128)),
                          w=[f"Kb{q4}"], key=f"kbl{q4}")
                      S.dma("pool", lambda e, b=b, q4=q4: e.dma_start(
                          out=Vb[:, q4 * 4:(q4 + 1) * 4, :],
                          in_=A["cv"][b, q4 * 512:(q4 + 1) * 512, :].rearrange("(j p) c -> p j c", p=128)),
                          w=[f"Vb{q4}"], key=f"vbl{q4}")
                  cnt = 0
                  for j in range(16):
                      for hp in range(6):
                          pbk = (cnt // 8) % 2
                          pos8 = cnt % 8
                          S.add("pe", lambda e, j=j, hp=hp, pbk=pbk, pos8=pos8: e.transpose(
                              out=psb[pbk][:, pos8 * 128:(pos8 + 1) * 128], in_=Kb[:, j, hp * 128:(hp + 1) * 128],
                              identity=ident[:, :]), r=[f"Kb{j // 4}", "ident"], w=[PSB[pbk]])
                          cnt += 1
                          if pos8 == 7:
                              first = cnt - 8
                              for q in range(8):
                                  jj_, hp_ = divmod(first + q, 6)
                              eng = "act" if (cnt // 8) % 2 == 0 else "dve"
                              blocks = [divmod(first + q, 6) for q in range(8)]
                              q = 0
                              while q < 8:
                                  j0, hp0 = blocks[q]
                                  q1 = q
                                  while q1 < 8 and blocks[q1][0] == j0:
                                      q1 += 1
                                  nrun = q1 - q
                                  if eng == "act":
                                      S.add("act", lambda e, pbk=pbk, q=q, nrun=nrun, j0=j0, hp0=hp0: e.activation(
                                          out=KTb[:, hp0:hp0 + nrun, j0, :],
                                          in_=psb[pbk][:, q * 128:(q + nrun) * 128].rearrange("p (a t) -> p a t", a=nrun),
                                          func=AF.Copy), r=[PSB[pbk]], w=["KTb"])
                                  else:
                                      S.add("dve", lambda e, pbk=pbk, q=q, nrun=nrun, j0=j0, hp0=hp0: e.tensor_copy(
                                          out=KTb[:, hp0:hp0 + nrun, j0, :],
                                          in_=psb[pbk][:, q * 128:(q + nrun) * 128].rearrange("p (a t) -> p a t", a=nrun)),
                                          r=[PSB[pbk]], w=["KTb"])
                                  q = q1
                  for gi, hs in enumerate(([0, 2, 4], [6, 8, 10], [1, 3, 5], [7, 9, 11])):
                      pb = gi % 2
                      for hq, h in enumerate(hs):
                          hp, base = h // 2, 64 * (h % 2)
                          for j in range(16):
                              S.add("pe", lambda e, hq=hq, j=j, hp=hp, base=base, pb=pb, b=b: e.matmul(
                                  psf[pb][:, hq * 128 + j * 8: hq * 128 + (j + 1) * 8],
                                  lhsT=KTb[base:base + 64, hp, j, :], rhs=QT[base:base + 64, hp, b * 8:(b + 1) * 8],
                                  start=True, stop=True), r=["KTb", "QT"], w=[PSF[pb]])
                      S.add("act", lambda e, pb=pb: e.activation(out=Eb[pb][:, 0:3, :].rearrange("p a t -> p (a t)"),
                                                                 in_=psf[pb][:, 0:384], func=AF.Exp, scale=0.125),
                            r=[PSF[pb]], w=[f"Eb{pb}"])
                      S.add("dve", lambda e, pb=pb: e.tensor_tensor(
                          out=PT[pb][:, 0:3, :].rearrange("p a (j t) -> p a j t", t=8),
                          in0=Eb[pb][:, 0:3, :].rearrange("p a (j t) -> p a j t", t=8),
                          in1=mask_sc[:, :, :].unsqueeze(1).to_broadcast([128, 3, 16, 8]), op=ALU.mult),
                          r=[f"Eb{pb}", "mask_sc"], w=[f"PT{pb}"])
                      for hq, h in enumerate(hs):
                          bank, hh = 4 + h // 6, h % 6
                          for j in range(16):
                              lhs = PT[pb][:, hq, j * 8:(j + 1) * 8]
                              S.add("pe", lambda e, lhs=lhs, j=j, h=h, bank=bank, hh=hh: e.matmul(
                                  psf[bank][0:8, hh * 65:hh * 65 + 64], lhsT=lhs, rhs=Vb[:, j, h * 64:(h + 1) * 64],
                                  start=(j == 0), stop=(j == 15)), r=[f"PT{pb}", f"Vb{j // 4}"], w=[PSF[bank]])
                          for j in range(16):
                              lhs = PT[pb][:, hq, j * 8:(j + 1) * 8]
                              S.add("pe", lambda e, lhs=lhs, j=j, bank=bank, hh=hh: e.matmul(
                                  psf[bank][0:8, hh * 65 + 64:hh * 65 + 65], lhsT=lhs, rhs=ones_c[:, 0:1],
                                  start=(j == 0), stop=(j == 15)), r=[f"PT{pb}", "ones_c"], w=[PSF[bank]])
                  for bk in range(2):
                      S.add("act", lambda e, bk=bk: e.activation(out=obs[:, bk * 390:(bk + 1) * 390], in_=psf[4 + bk][0:8, 0:390],
                                                                 func=AF.Copy), r=[PSF[4 + bk]], w=["obs", "rtmpa", "rtmpb"])
                  for bk in range(2):
                      S.add("pe", lambda e, bk=bk, b=b: e.matmul(psf[2 + bk][:, 0:390], lhsT=wshift[0:8, 128 - 8 * b:256 - 8 * b],
                                                                 rhs=obs[:, bk * 390:(bk + 1) * 390], start=(b == 0), stop=False,
                                                                 skip_group_check=True),
                            r=["wshift", "obs"], w=[PSF[2 + bk]])
              for gi, hs in enumerate(([0, 2, 4], [6, 8, 10], [1, 3, 5], [7, 9, 11])):
                  pb = gi % 2
                  for hq, h in enumerate(hs):
                      hp, base = h // 2, 64 * (h % 2)
                      S.add("pe", lambda e, hq=hq, hp=hp, base=base, pb=pb: e.matmul(
                          psf[pb][:, hq * 128:(hq + 1) * 128], lhsT=KTs[base:base + 64, hp, :], rhs=QT[base:base + 64, hp, :],
                          start=True, stop=True), r=["KTs", "QT"], w=[PSF[pb]])
                  S.add("act", lambda e, pb=pb: e.activation(out=Eb[pb][:, 0:3, :].rearrange("p a t -> p (a t)"),
                                                             in_=psf[pb][:, 0:384], func=AF.Exp, scale=0.125),
                        r=[PSF[pb]], w=[f"Eb{pb}"])
                  S.add("dve", lambda e, pb=pb: e.tensor_tensor(out=PT[pb][:, 0:3, :], in0=Eb[pb][:, 0:3, :],
                                                                in1=mask_sn[:, :].unsqueeze(1).to_broadcast([128, 3, 128]),
                                                                op=ALU.mult), r=[f"Eb{pb}", "mask_sn"], w=[f"PT{pb}"])
                  for hq, h in enumerate(hs):
                      bank, hh = 2 + h // 6, h % 6
                      S.add("pe", lambda e, pb=pb, hq=hq, h=h, bank=bank, hh=hh: e.matmul(
                          psf[bank][:, hh * 65:(hh + 1) * 65], lhsT=PT[pb][:, hq, :], rhs=VAs[:, h, :], start=(nsb_run == 0), stop=True,
                          skip_group_check=True), r=[f"PT{pb}", "VAs"], w=[PSF[bank]])
              attn_finish()
              back(slot, g)


        if do_phase2:
            S.barrier()
            wflat = w_in_bf[:, :, :].rearrange("p a n -> p (a n)")
            wq_bf = wflat[:, 0:16384].rearrange("p (a n) -> p a n", a=8)
            skT = wflat[:, 16384:18432].rearrange("p (g k) -> p g k", g=16)
            g2b = wflat[:, 18432:19456]
            gfb = wflat[:, 19456:20480]
            hgb = wflat[:, 20480:21504]
            T2 = wflat[:, 21504:22528].rearrange("p (a t) -> p a t", a=8)
            qpT = wflat[:, 22528:24576].rearrange("p (g t) -> p g t", g=16)
            NG = 16
            G = [SEQB[:, i * 2048:(i + 1) * 2048] for i in range(NG)]
            DG = [SEQB[:, 32768 + i * 128:32768 + (i + 1) * 128] for i in range(8)]
            JK = [junk[:, :], xb[:, :], TT[:, :, :].rearrange("p a t -> p (a t)"), SEQB[:, 33792:34816]]
            wo32 = w_out_bf[:, :, :].rearrange("p a n -> p (a n)").bitcast(F32)
            s_sb = wo32[:, 0:2048].rearrange("p (g k) -> p g k", g=16)
            cand = wo32[:, 2048:4096].rearrange("p (h k) -> p h k", h=8)
            c2 = attn[:, 0:256]
            s2 = attn[:, 256:384]
            sv = attn[:, 384:640].rearrange("p (g k) -> p g k", g=16)
            top = attn[:, 640:768].rearrange("p (h k) -> p h k", h=8)
            mx32 = mixin[:, :].bitcast(F32)
            gsm = mx32[:, 0:128]
            dots = mx32[:, 128:256]
            actv = mx32[:, 256:384]
            eidf = mx32[:, 384:512]
            vu = vf[:, :].bitcast(U32)
            si = vu[:, 0:256].rearrange("p (g k) -> p g k", g=16)
            pos = vu[:, 256:384]
            a_i = vu[:, 384:512]
            b_i = vu[:, 512:640]
            eid_i = vu[:, 640:768]
            sif = cf[:, 0:256].rearrange("p (h q k) -> p h q k", h=8, q=2)
            af = cf[:, 256:384].rearrange("p (h k) -> p h k", h=8)
            bf = cf[:, 384:512].rearrange("p (h k) -> p h k", h=8)
            isel = cf[:, 512:640].rearrange("p (h k) -> p h k", h=8)
            jsel = cf[:, 640:768].rearrange("p (h k) -> p h k", h=8)
            eq = qk[:, 0:1024].rearrange("p (h k a) -> p h k a", h=4, k=16)
            den8 = rden[:, 0:8]
            iota16 = zb[0][:, 0:16]

            NCH = 1 if small_io else 32
            for c in range(NCH):
                stg = c % 4
                stv = SEQB[:, stg * 8192:(stg + 1) * 8192].rearrange("p (r n) -> p r n", r=4)
                S.dma("pool", lambda e, c=c, stv=stv: e.dma_start(
                    out=stv[:, :, 0:1024], in_=A["pu"][c * 512:(c + 1) * 512, :].rearrange("(p r) d -> p r d", r=4)),
                    w=[f"stg{stg}u"], key=f"uvla{stg}")
                S.dma("pool", lambda e, c=c, stv=stv: e.dma_start(
                    out=stv[:, :, 1024:2048], in_=A["pv"][c * 512:(c + 1) * 512, :].rearrange("(p r) d -> p r d", r=4)),
                    w=[f"stg{stg}v"], key=f"uvlb{stg}")
                S.dma("sp", lambda e, c=c, stv=stv: e.dma_start(
                    out=uvd[c * 512:(c + 1) * 512, :].rearrange("(p r) n -> p r n", r=4), in_=stv),
                    r=[f"stg{stg}u", f"stg{stg}v"], w=[f"uvd{c}"], key=f"uvs{stg}")
            for kc in range(8):
                S.dma("pool", lambda e, kc=kc: e.dma_start(out=wq_bf[:, kc, :], in_=A["wq"][kc * 128:(kc + 1) * 128, :]),
                      w=["wq_bf"], key=f"wl{kc % 4}")
            S.dma("pool", lambda e: e.dma_start(out=g2b, in_=A["g2"].partition_broadcast(128)), w=["g2b"], key="cp2a")
            S.dma("pool", lambda e: e.dma_start(out=gfb, in_=A["gf"].partition_broadcast(128)), w=["gfb"], key="cp2b")
            S.dma("sp", lambda e: e.dma_start(out=iota16, in_=A["iota16"]), w=["iota16"], key="cp2c")
            S.dma("pool", lambda e: e.dma_start(out=qpT, in_=A["sk"].rearrange("g k c -> k g c")), w=["qpT"], key="cp2d")
            for g16 in range(16):
                pbk, pos8 = g16 // 8, g16 % 8
                S.add("pe", lambda e, g16=g16, pbk=pbk, pos8=pos8: e.transpose(
                    out=psb[pbk][:, pos8 * 128:(pos8 + 1) * 128], in_=qpT[:, g16, :], identity=ident[:, :]),
                    r=["qpT", "ident"], w=[PSB[pbk]])
            for pbk in range(2):
                S.add("act", lambda e, pbk=pbk: e.activation(
                    out=skT[:, pbk * 8:(pbk + 1) * 8, :].rearrange("p g k -> p (g k)"), in_=psb[pbk][:, :], func=AF.Copy),
                    r=[PSB[pbk]], w=["skT"])
            S.add("pool", None, r=[f"uvd{c}" for c in range(NCH)])

            def peer_tile(gidx, dst):
                slot = gidx % 2
                X = xin[slot]
                xn = f"xin{slot}"
                S.dma("sp", lambda e: e.dma_start(out=X[:, :], in_=h1d[gidx, :, :]), r=[f"h1d{gidx}"], w=[xn], key=xn)
                S.add("dve", lambda e: e.scalar_tensor_tensor(out=JK[0], in0=X[:, :], scalar=1.0, in1=X[:, :],
                                                              op0=ALU.mult, op1=ALU.mult, accum_out=stat[:, 6:7]),
                      r=[xn], w=["jk0", "st6"])
                rstd_ops(6, 7, D)
                S.add("dve", lambda e: e.tensor_tensor(out=hgb, in0=X[:, :], in1=g2b, op=ALU.mult), r=[xn, "g2b"], w=["hgb"])
                for kc in range(8):
                    S.add("pe", lambda e, kc=kc: e.transpose(out=psb[0][:, kc * 128:(kc + 1) * 128],
                                                             in_=hgb[:, kc * 128:(kc + 1) * 128], identity=ident[:, :]),
                          r=["hgb", "ident"], w=["ps6"])
                S.add("act", lambda e: e.activation(out=T2.rearrange("p a t -> p (a t)"), in_=psb[0][:, :], func=AF.Copy),
                      r=["ps6"], w=["T2"])
                for kc in range(8):
                    S.add("pe", lambda e, kc=kc: e.transpose(out=psb[1][:, kc * 128:(kc + 1) * 128], in_=T2[:, kc, :],
                                                             identity=ident[:, :]), r=["T2", "ident"], w=["ps7"])
                for g16 in range(16):
                    bk, jj = g16 // 4, g16 % 4
                    for kc in range(8):
                        S.add("pe", lambda e, g16=g16, bk=bk, jj=jj, kc=kc: e.matmul(
                            psf[bk][:, jj * 128:(jj + 1) * 128], lhsT=wq_bf[:, kc, g16 * 128:(g16 + 1) * 128], rhs=T2[:, kc, :],
                            start=(kc == 0), stop=(kc == 7)), r=["wq_bf", "T2"], w=[PSF[bk]])
                for bk in range(4):
                    eng = "act" if bk % 2 == 0 else "dve"
                    if eng == "act":
                        S.add("act", lambda e, bk=bk: e.activation(out=qpT[:, bk * 4:(bk + 1) * 4, :].rearrange("p g t -> p (g t)"),
                                                                   in_=psf[bk][:, :], func=AF.Copy), r=[PSF[bk]], w=[f"qpT{bk}"])
                    else:
                        S.add("dve", lambda e, bk=bk: e.tensor_copy(out=qpT[:, bk * 4:(bk + 1) * 4, :].rearrange("p g t -> p (g t)"),
                                                                    in_=psf[bk][:, :]), r=[PSF[bk]], w=[f"qpT{bk}"])
                for g16 in range(16):
                    bk, jj = g16 // 4, g16 % 4
                    S.add("pe", lambda e, g16=g16, bk=bk, jj=jj: e.matmul(
                        psf[bk][:, jj * 128:(jj + 1) * 128], lhsT=qpT[:, g16, :], rhs=skT[:, g16, :], start=True, stop=True),
                        r=[f"qpT{bk}", "skT"], w=[PSF[bk]])
                for bk in range(4):
                    if True:
                        S.add("act", lambda e, bk=bk: e.activation(out=s_sb[:, bk * 4:(bk + 1) * 4, :].rearrange("p g k -> p (g k)"),
                                                                   in_=psf[bk][:, :], func=AF.Copy, scale=stat[:, 7:8]),
                              r=[PSF[bk], "st7"], w=[f"s_sb{bk}"])
                    else:
                        S.add("dve", lambda e, bk=bk: e.tensor_scalar(out=s_sb[:, bk * 4:(bk + 1) * 4, :].rearrange("p g k -> p (g k)"),
                                                                      in0=psf[bk][:, :], scalar1=stat[:, 7:8], scalar2=None,
                                                                      op0=ALU.mult), r=[PSF[bk], "st7"], w=[f"s_sb{bk}"])
                for g16 in range(16):
                    sn = f"s_sb{g16 // 4}"
                    S.add("dve", lambda e, g16=g16: e.max(out=sv[:, g16, 0:8], in_=s_sb[:, g16, :]), r=[sn], w=[f"sva{g16}"])
                    S.add("dve", lambda e, g16=g16: e.max_index(out=si[:, g16, 0:8], in_max=sv[:, g16, 0:8], in_values=s_sb[:, g16, :]),
                          r=[sn, f"sva{g16}"], w=[f"sia{g16}"])
                    S.add("dve", lambda e, g16=g16: e.match_replace(out=s2, in_to_replace=sv[:, g16, 0:8], in_values=s_sb[:, g16, :],
                                                                    imm_value=NEGBIG), r=[sn, f"sva{g16}"], w=["s2"])
                    S.add("dve", lambda e, g16=g16: e.max(out=sv[:, g16, 8:16], in_=s2), r=["s2"], w=[f"svb{g16}"])
                    S.add("dve", lambda e, g16=g16: e.max_index(out=si[:, g16, 8:16], in_max=sv[:, g16, 8:16], in_values=s2),
                          r=["s2", f"svb{g16}"], w=[f"sib{g16}"])
                svall = [f"sva{i}" for i in range(16)] + [f"svb{i}" for i in range(16)]
                siall = [f"sia{i}" for i in range(16)] + [f"sib{i}" for i in range(16)]
                sv4 = sv.rearrange("p (h q) k -> p h q k", q=2)
                S.add("dve", lambda e: e.tensor_tensor(
                    out=cand.rearrange("p h (a b) -> p h a b", a=16),
                    in0=sv4[:, :, 0, :].unsqueeze(3).to_broadcast([128, 8, 16, 16]),
                    in1=sv4[:, :, 1, :].unsqueeze(2).to_broadcast([128, 8, 16, 16]), op=ALU.add), r=svall, w=["cand"])
                for h in range(8):
                    S.add("dve", lambda e, h=h: e.max(out=top[:, h, 0:8], in_=cand[:, h, :]), r=["cand"], w=[f"topa{h}"])
                    S.add("dve", lambda e, h=h: e.max_index(out=pos[:, h * 16:h * 16 + 8], in_max=top[:, h, 0:8], in_values=cand[:, h, :]),
                          r=["cand", f"topa{h}"], w=[f"posa{h}"])
                    S.add("dve", lambda e, h=h: e.match_replace(out=c2, in_to_replace=top[:, h, 0:8], in_values=cand[:, h, :],
                                                                imm_value=NEGBIG), r=["cand", f"topa{h}"], w=["c2"])
                    S.add("dve", lambda e, h=h: e.max(out=top[:, h, 8:16], in_=c2), r=["c2"], w=[f"topb{h}"])
                    S.add("dve", lambda e, h=h: e.max_index(out=pos[:, h * 16 + 8:h * 16 + 16], in_max=top[:, h, 8:16], in_values=c2),
                          r=["c2", f"topb{h}"], w=[f"posb{h}"])
                topall = [f"topa{h}" for h in range(8)] + [f"topb{h}" for h in range(8)]
                posall = [f"posa{h}" for h in range(8)] + [f"posb{h}" for h in range(8)]
                g3 = gsm.rearrange("p (h k) -> p h k", h=8)
                S.add("dve", lambda e: e.tensor_tensor(out=g3, in0=top, in1=top[:, :, 0:1].to_broadcast([128, 8, 16]), op=ALU.subtract),
                      r=topall, w=["gsm"])
                S.add("act", lambda e: e.activation(out=gsm, in_=gsm, func=AF.Exp), r=["gsm"], w=["gsm"])
                S.add("dve", lambda e: e.tensor_reduce(out=den8, in_=g3, axis=AX.X, op=ALU.add), r=["gsm"], w=["den8"])
                S.add("dve", lambda e: e.reciprocal(out=den8, in_=den8), r=["den8"], w=["den8"])
                S.add("dve", lambda e: e.tensor_tensor(out=g3, in0=g3, in1=den8.unsqueeze(2).to_broadcast([128, 8, 16]), op=ALU.mult),
                      r=["gsm", "den8"], w=["gsm"])
                S.add("dve", lambda e: e.tensor_single_scalar(out=a_i, in_=pos, scalar=4, op=ALU.logical_shift_right), r=posall, w=["a_i"])
                S.add("dve", lambda e: e.tensor_single_scalar(out=b_i, in_=pos, scalar=15, op=ALU.bitwise_and), r=posall, w=["b_i"])
                S.add("dve", lambda e: e.tensor_copy(out=af.rearrange("p h k -> p (h k)"), in_=a_i), r=["a_i"], w=["af"])
                S.add("dve", lambda e: e.tensor_copy(out=bf.rearrange("p h k -> p (h k)"), in_=b_i), r=["b_i"], w=["bf"])
                S.add("dve", lambda e: e.tensor_copy(out=cf[:, 0:256], in_=vu[:, 0:256]), r=siall, w=["sif"])
                for (xf_, q_, osel, oname) in ((af, 0, isel, "isel"), (bf, 1, jsel, "jsel")):
                    for hh in range(2):
                        hs = slice(hh * 4, hh * 4 + 4)
                        S.add("dve", lambda e, xf_=xf_, hs=hs: e.tensor_tensor(
                            out=eq, in0=xf_[:, hs, :].unsqueeze(3).to_broadcast([128, 4, 16, 16]),
                            in1=iota16.unsqueeze(1).unsqueeze(1).to_broadcast([128, 4, 16, 16]), op=ALU.is_equal),
                            r=["af", "bf", "iota16"], w=["eq"])
                        S.add("dve", lambda e, q_=q_, hs=hs: e.tensor_tensor(
                            out=eq, in0=eq, in1=sif[:, hs, q_, :].unsqueeze(2).to_broadcast([128, 4, 16, 16]), op=ALU.mult),
                            r=["eq", "sif"], w=["eq"])
                        S.add("dve", lambda e, osel=osel, hs=hs: e.tensor_reduce(out=osel[:, hs, :], in_=eq, axis=AX.X, op=ALU.add),
                              r=["eq"], w=[f"{oname}{hh}"])
                S.add("dve", lambda e: e.scalar_tensor_tensor(out=eidf, in0=isel.rearrange("p h k -> p (h k)"), scalar=128.0,
                                                              in1=jsel.rearrange("p h k -> p (h k)"), op0=ALU.mult, op1=ALU.add),
                      r=["isel0", "isel1", "jsel0", "jsel1"], w=["eidf"])
                S.add("dve", lambda e: e.tensor_copy(out=eid_i, in_=eidf), r=["eidf"], w=["eid_i"])

                def gather(hk):
                    sl = hk % NG
                    S.dma("pool", lambda e, hk=hk, sl=sl: e.indirect_dma_start(
                        out=G[sl], out_offset=None, in_=uvd[:, :],
                        in_offset=bass.IndirectOffsetOnAxis(ap=eid_i[:, hk:hk + 1], axis=0)),
                        r=["eid_i"], w=[f"G{sl}"], key=f"ga{sl}")

                def dot(hk):
                    sl = hk % NG
                    S.add("dve", lambda e, hk=hk, sl=sl: e.scalar_tensor_tensor(
                        out=JK[hk % 4], in0=G[sl][:, 0:1024], scalar=1.0, in1=psb[1][:, :], op0=ALU.mult, op1=ALU.mult,
                        accum_out=dots[:, hk:hk + 1]), r=[f"G{sl}", "ps7"], w=[f"jk{hk % 4}", f"dot{hk}"])
                    S.add("act", lambda e, hk=hk: e.activation(out=actv[:, hk:hk + 1], in_=dots[:, hk:hk + 1], func=AF.Gelu,
                                                               scale=stat[:, 7:8]), r=[f"dot{hk}", "st7"], w=[f"actv{hk}"])

                def acc(hk):
                    sl = hk % NG
                    dsl = hk % 8
                    S.add("dve", lambda e, hk=hk, dsl=dsl: e.tensor_scalar(
                        out=DG[dsl], in0=ident[:, :], scalar1=gsm[:, hk:hk + 1], scalar2=actv[:, hk:hk + 1],
                        op0=ALU.mult, op1=ALU.mult), r=["ident", "gsm", f"actv{hk}"], w=[f"DG{dsl}"])
                    for nb in range(2):
                        S.add("pe", lambda e, hk=hk, sl=sl, dsl=dsl, nb=nb: e.matmul(
                            psf[4 + nb][:, :], lhsT=DG[dsl], rhs=G[sl][:, 1024 + nb * 512:1024 + (nb + 1) * 512],
                            start=(hk == 0), stop=(hk == 127)), r=[f"DG{dsl}", f"G{sl}"], w=[PSF[4 + nb]])

                for hk in range(NG):
                    gather(hk)
                for hk in range(128):
                    dot(hk)
                    if hk >= 1:
                        acc(hk - 1)
                        if hk - 1 + NG < 128:
                            gather(hk - 1 + NG)
                acc(127)
                for nb in range(2):
                    S.add("dve", lambda e, nb=nb: e.tensor_tensor(out=X[:, nb * 512:(nb + 1) * 512], in0=psf[4 + nb][:, :],
                                                                  in1=X[:, nb * 512:(nb + 1) * 512], op=ALU.add),
                          r=[PSF[4 + nb], xn], w=[xn])
                S.add("dve", lambda e: e.scalar_tensor_tensor(out=JK[0], in0=X[:, :], scalar=1.0, in1=X[:, :],
                                                              op0=ALU.mult, op1=ALU.mult, accum_out=stat[:, 8:9]),
                      r=[xn], w=["jk0", "st8"])
                rstd_ops(8, 9, D)
                S.add("dve", lambda e: e.scalar_tensor_tensor(out=X[:, :], in0=X[:, :], scalar=stat[:, 9:10], in1=gfb,
                                                              op0=ALU.mult, op1=ALU.mult), r=[xn, "st9", "gfb"], w=[xn])
                S.dma("sp", lambda e: e.dma_start(out=dst, in_=X[:, :]), r=[xn], key=f"yo{slot}")

            for g in range(n_ptiles):
                s_, ti = divmod(g, NT)
                peer_tile(g, A["yp"][s_, ti * 128:(ti + 1) * 128, :])
            if do_sample and not sample_front_only:
                peer_tile(NSEQ * NT, A["ys"])

        if max_ops is not None:
            S.ops = S.ops[:max_ops]
            S.ops = [o for i, o in enumerate(S.ops) if i not in skip_ops]
        S.barrier()
        S.emit(nc, st)
    return nc


def shard_inputs(inp):
    consts = make_consts()
    f = lambda a: np.ascontiguousarray(a, dtype=np.float32)
    common = {
        "g1": f(inp["norm1_g"][0]), "w_in": f(inp["w_in"][0]), "conv_w": f(inp["conv_w"][0]),
        "go": f(np.concatenate([inp["attn_out_g"][0], inp["conv_out_g"][0]])), "w_out": f(inp["w_out"][0]),
        "g2": f(inp["norm2_g"][0]), "wq": f(inp["peer_wq"][0]), "sk": f(inp["peer_sub_keys"][0].reshape(16, 128, 128)),
        "pu": f(inp["peer_u"][0]), "pv": f(inp["peer_v"][0]), "gf": f(inp["final_norm_g"]),
    }
    common.update(consts)
    maps = []
    for c in range(NCORES):
        m = dict(common)
        m["xp"] = f(inp["x_prompt"][NSEQ * c:NSEQ * (c + 1)])
        m["xs"] = f(inp["x_sample"][NSB * c:NSB * (c + 1)].reshape(128, D))
        m["ck"] = f(inp["cache_win_k"][0, NSB * c:NSB * (c + 1)].reshape(NSB, 2048, 768))
        m["cv"] = f(inp["cache_win_v"][0, NSB * c:NSB * (c + 1)].reshape(NSB, 2048, 768))
        m["sc"] = f(inp["state_conv"][0, NSB * c:NSB * (c + 1)].reshape(32, 256))
        maps.append(m)
    return maps


def gather_outputs(results):
    cat = lambda k: np.concatenate([np.asarray(r[k]) for r in results], axis=0)
    yp = cat("yp")
    ys = cat("ys").reshape(128, 8, D)
    kp = cat("kp").reshape(1, 32, SEQ, 12, 64)
    vp = cat("vp").reshape(1, 32, SEQ, 12, 64)
    cp = cat("cp").reshape(1, 32, 2, 256)
    ks = cat("ks").reshape(1, 128, 8, 12, 64)
    vs = cat("vs").reshape(1, 128, 8, 12, 64)
    cs = cat("cs").reshape(1, 128, 2, 256)
    return tuple(np.ascontiguousarray(a, dtype=np.float32) for a in (yp, ys, kp, vp, cp, ks, vs, cs))


def kernel(**inputs):
    inputs = {k: np.asarray(v) for k, v in inputs.items()}
    nc = build()
    maps = shard_inputs(inputs)
    res = run_bass_kernel_spmd(nc, maps, core_ids=list(range(NCORES)))
    return gather_outputs(res.results)
```
